# Optimizing a Trainium2 kernel written in Bass

```python
import math
import jax
import jax.numpy as jnp
from jax import lax
import numpy as np

D_MODEL = 1024
BATCH = 2
SEQ = 8192
DEPTH = 2

CHUNK = 64
Q_BLOCK = 128
HEAD_DIM = 64
N_HEADS_SB = 8
N_HEADS_DIFF = 4
N_HEADS_CHUNK = D_MODEL // HEAD_DIM
D_SB = N_HEADS_SB * HEAD_DIM
D_DIFF = N_HEADS_DIFF * 2 * HEAD_DIM
D_MIX = D_SB + D_DIFF
D_IN_EVEN = 3 * D_SB + 3 * D_DIFF
ROPE_THETA = 500000.0
ROT_DIM = HEAD_DIM // 4
LEFT_CHUNKS = 8
BAND = (LEFT_CHUNKS + 1) * CHUNK
REL_CLIP = 128
N_REL = 2 * REL_CLIP + 1
D_FF_DENSE = 2816
N_EXPERTS = 8
TOP_K = 2
D_FF_EXPERT = 3584
RMS_EPS = 1e-6
N_EVEN = (DEPTH + 1) // 2
N_ODD = DEPTH // 2

kernel_name = 'hybrid_stickbreak_diff_chunkband_moe_trunk'

F32 = jnp.float32


def rms_norm(x, g):
    xf = x.astype(F32)
    y = xf * lax.rsqrt(jnp.mean(xf * xf, axis=-1, keepdims=True) + RMS_EPS)
    return (y * g.astype(F32)).astype(x.dtype)


def rope_tables(positions):
    inv_freq = ROPE_THETA ** (-jnp.arange(0, ROT_DIM, 2, dtype=F32) / ROT_DIM)
    ang = positions.astype(F32)[..., None] * inv_freq
    return jnp.cos(ang)[:, None], jnp.sin(ang)[:, None]


def partial_rope(x, cos, sin):
    half = ROT_DIM // 2
    x1 = x[..., :half].astype(F32)
    x2 = x[..., half:ROT_DIM].astype(F32)
    rot = jnp.concatenate([x1 * cos - x2 * sin, x2 * cos + x1 * sin], axis=-1)
    return jnp.concatenate([rot.astype(x.dtype), x[..., ROT_DIM:]], axis=-1)


def split_heads(t, n):
    b, s, _ = t.shape
    return t.reshape(b, s, n, -1).transpose(0, 2, 1, 3)


def merge_heads(o):
    b, h, s, d = o.shape
    return o.transpose(0, 2, 1, 3).reshape(b, s, h * d)


def to_query_blocks(q):
    b, h, s, d = q.shape
    return jnp.moveaxis(q.reshape(b, h, s // Q_BLOCK, Q_BLOCK, d), 2, 0)


def from_query_blocks(o):
    nb, b, h, qb, d = o.shape
    return jnp.moveaxis(o, 0, 2).reshape(b, h, nb * qb, d)


def stick_breaking_attention(q, k, v):
    s_len = q.shape[2]
    nb = s_len // Q_BLOCK
    scale = HEAD_DIM ** -0.5
    k_idx = jnp.arange(s_len)

    def block(args):
        q_blk, start = args
        z = jnp.einsum('bhqd,bhkd->bhqk', q_blk, k, preferred_element_type=F32) * scale
        t_idx = start + jnp.arange(Q_BLOCK)
        past = k_idx[None, :] < t_idx[:, None]
        log_stay = jnp.where(past, jax.nn.log_sigmoid(-z), 0.0)
        between = lax.cumsum(log_stay, axis=3, reverse=True) - log_stay
        w = jnp.where(past, jnp.exp(jax.nn.log_sigmoid(z) + between), 0.0)
        return jnp.einsum('bhqk,bhkd->bhqd', w.astype(v.dtype), v)

    out = lax.map(block, (to_query_blocks(q), jnp.arange(nb, dtype=jnp.int32) * Q_BLOCK))
    return from_query_blocks(out)


def differential_attention(q, k, v, lam):
    b, h, _, s_len, d = q.shape
    nb = s_len // Q_BLOCK
    scale = HEAD_DIM ** -0.5
    k_chunk = jnp.arange(s_len) // CHUNK
    q_blocks = jnp.moveaxis(q.reshape(b, h, 2, nb, Q_BLOCK, d), 3, 0)

    def block(args):
        q_blk, start = args
        sc = jnp.einsum('bhmqd,bhmkd->bhmqk', q_blk, k, preferred_element_type=F32) * scale
        t_chunk = (start + jnp.arange(Q_BLOCK)) // CHUNK
        visible = k_chunk[None, :] <= t_chunk[:, None]
        p = jax.nn.softmax(jnp.where(visible, sc, -jnp.inf), axis=-1)
        w = p[:, :, 0] - lam * p[:, :, 1]
        return jnp.einsum('bhqk,bhkv->bhqv', w.astype(v.dtype), v)

    out = lax.map(block, (q_blocks, jnp.arange(nb, dtype=jnp.int32) * Q_BLOCK))
    return from_query_blocks(out)


def chunk_band_attention(q, k, v, rel_table):
    b, h, s_len, d = q.shape
    nc = s_len // CHUNK
    pad = LEFT_CHUNKS * CHUNK
    scale = HEAD_DIM ** -0.5
    k_pad = jnp.pad(k, ((0, 0), (0, 0), (pad, 0), (0, 0)))
    v_pad = jnp.pad(v, ((0, 0), (0, 0), (pad, 0), (0, 0)))
    i = jnp.arange(CHUNK)
    j = jnp.arange(BAND)
    rel = jnp.clip(pad + i[:, None] - j[None, :], -REL_CLIP, REL_CLIP) + REL_CLIP
    bias = rel_table[:, rel].astype(F32)
    q_chunks = jnp.moveaxis(q.reshape(b, h, nc, CHUNK, d), 2, 0)

    def chunk(args):
        q_c, c = args
        start = c * CHUNK
        k_band = lax.dynamic_slice_in_dim(k_pad, start, BAND, axis=2)
        v_band = lax.dynamic_slice_in_dim(v_pad, start, BAND, axis=2)
        sc = jnp.einsum('bhqd,bhkd->bhqk', q_c, k_band, preferred_element_type=F32) * scale + bias
        valid = (start - pad + j) >= 0
        p = jax.nn.softmax(jnp.where(valid[None, None, None, :], sc, -jnp.inf), axis=-1)
        return jnp.einsum('bhqk,bhkd->bhqd', p.astype(v.dtype), v_band)

    out = lax.map(chunk, (q_chunks, jnp.arange(nc, dtype=jnp.int32)))
    return jnp.moveaxis(out, 0, 2).reshape(b, h, s_len, d)


def even_mixer(h, cos, sin, w_in, q_norm, k_norm, lam_q1, lam_k1, lam_q2, lam_k2, subln, w_out, layer):
    b, s, _ = h.shape
    proj = h @ w_in
    cuts = [D_SB, 2 * D_SB, 3 * D_SB, 3 * D_SB + D_DIFF, 3 * D_SB + 2 * D_DIFF]
    q_sb, k_sb, v_sb, q_df, k_df, v_df = jnp.split(proj, cuts, axis=-1)
    o_sb = stick_breaking_attention(split_heads(q_sb, N_HEADS_SB), split_heads(k_sb, N_HEADS_SB),
                                    split_heads(v_sb, N_HEADS_SB))
    qd = partial_rope(rms_norm(split_heads(q_df, 2 * N_HEADS_DIFF), q_norm), cos, sin)
    kd = partial_rope(rms_norm(split_heads(k_df, 2 * N_HEADS_DIFF), k_norm), cos, sin)
    qd = qd.reshape(b, N_HEADS_DIFF, 2, s, HEAD_DIM)
    kd = kd.reshape(b, N_HEADS_DIFF, 2, s, HEAD_DIM)
    vd = split_heads(v_df, N_HEADS_DIFF)
    lam_init = 0.8 - 0.6 * math.exp(-0.3 * layer)
    lam = (jnp.exp(jnp.sum(lam_q1.astype(F32) * lam_k1.astype(F32)))
           - jnp.exp(jnp.sum(lam_q2.astype(F32) * lam_k2.astype(F32))) + lam_init)
    o_df = rms_norm(differential_attention(qd, kd, vd, lam), subln) * (1.0 - lam_init)
    merged = jnp.concatenate([merge_heads(o_sb), merge_heads(o_df)], axis=-1)
    return merged @ w_out


def odd_mixer(h, w_qkv, q_norm, k_norm, rel_table, w_out):
    q, k, v = jnp.split(h @ w_qkv, 3, axis=-1)
    q = rms_norm(split_heads(q, N_HEADS_CHUNK), q_norm)
    k = rms_norm(split_heads(k, N_HEADS_CHUNK), k_norm)
    o = chunk_band_attention(q, k, split_heads(v, N_HEADS_CHUNK), rel_table)
    return merge_heads(o) @ w_out


def swiglu(h, w_gate, w_up, w_down):
    return (jax.nn.silu(h @ w_gate) * (h @ w_up)) @ w_down


def moe_swiglu(h, w_router, we_gate, we_up, we_down):
    b, s, d = h.shape
    t = h.reshape(b * s, d)
    logits = (t @ w_router).astype(F32)
    top_val, top_idx = lax.top_k(logits, TOP_K)
    gates = jax.nn.softmax(top_val, axis=-1)
    combine = jnp.einsum('tk,tke->te', gates, jax.nn.one_hot(top_idx, N_EXPERTS, dtype=F32))
    y = jnp.zeros_like(t)
    for e in range(N_EXPERTS):
        y = y + combine[:, e:e + 1].astype(t.dtype) * swiglu(t, we_gate[e], we_up[e], we_down[e])
    return y.reshape(b, s, d)


def setup_inputs(seed: int = 0) -> dict:
    key = jax.random.key(seed)
    ks = jax.random.split(key, 32)

    def nrm(k, shape, scale):
        return jax.random.normal(k, shape, F32) * scale

    def gain(k, shape):
        return 1.0 + 0.02 * jax.random.normal(k, shape, F32)

    x = nrm(ks[0], (BATCH, SEQ, D_MODEL), 1.0)
    offsets = jax.random.randint(ks[1], (BATCH, 1), 0, 16, dtype=jnp.int32) * CHUNK
    positions = offsets + jnp.arange(SEQ, dtype=jnp.int32)[None, :]
    return {
        'x': x,
        'positions': positions,
        'ev_attn_norm': gain(ks[2], (N_EVEN, D_MODEL)),
        'ev_w_in': nrm(ks[3], (N_EVEN, D_MODEL, D_IN_EVEN), D_MODEL ** -0.5),
        'ev_q_norm': gain(ks[4], (N_EVEN, HEAD_DIM)),
        'ev_k_norm': gain(ks[5], (N_EVEN, HEAD_DIM)),
        'ev_lambda_q1': nrm(ks[6], (N_EVEN, HEAD_DIM), 0.1),
        'ev_lambda_k1': nrm(ks[7], (N_EVEN, HEAD_DIM), 0.1),
        'ev_lambda_q2': nrm(ks[8], (N_EVEN, HEAD_DIM), 0.1),
        'ev_lambda_k2': nrm(ks[9], (N_EVEN, HEAD_DIM), 0.1),
        'ev_subln': gain(ks[10], (N_EVEN, 2 * HEAD_DIM)),
        'ev_w_out': nrm(ks[11], (N_EVEN, D_MIX, D_MODEL), D_MIX ** -0.5),
        'ev_ffn_norm': gain(ks[12], (N_EVEN, D_MODEL)),
        'ev_w_gate': nrm(ks[13], (N_EVEN, D_MODEL, D_FF_DENSE), D_MODEL ** -0.5),
        'ev_w_up': nrm(ks[14], (N_EVEN, D_MODEL, D_FF_DENSE), D_MODEL ** -0.5),
        'ev_w_down': nrm(ks[15], (N_EVEN, D_FF_DENSE, D_MODEL), D_FF_DENSE ** -0.5),
        'od_attn_norm': gain(ks[16], (N_ODD, D_MODEL)),
        'od_w_qkv': nrm(ks[17], (N_ODD, D_MODEL, 3 * N_HEADS_CHUNK * HEAD_DIM), D_MODEL ** -0.5),
        'od_q_norm': gain(ks[18], (N_ODD, HEAD_DIM)),
        'od_k_norm': gain(ks[19], (N_ODD, HEAD_DIM)),
        'od_rel_bias': nrm(ks[20], (N_ODD, N_HEADS_CHUNK, N_REL), 0.5),
        'od_w_out': nrm(ks[21], (N_ODD, N_HEADS_CHUNK * HEAD_DIM, D_MODEL), (N_HEADS_CHUNK * HEAD_DIM) ** -0.5),
        'od_ffn_norm': gain(ks[22], (N_ODD, D_MODEL)),
        'od_router': nrm(ks[23], (N_ODD, D_MODEL, N_EXPERTS), D_MODEL ** -0.5),
        'od_we_gate': nrm(ks[24], (N_ODD, N_EXPERTS, D_MODEL, D_FF_EXPERT), D_MODEL ** -0.5),
        'od_we_up': nrm(ks[25], (N_ODD, N_EXPERTS, D_MODEL, D_FF_EXPERT), D_MODEL ** -0.5),
        'od_we_down': nrm(ks[26], (N_ODD, N_EXPERTS, D_FF_EXPERT, D_MODEL), D_FF_EXPERT ** -0.5),
    }


def reference(x, positions, ev_attn_norm, ev_w_in, ev_q_norm, ev_k_norm, ev_lambda_q1, ev_lambda_k1,
              ev_lambda_q2, ev_lambda_k2, ev_subln, ev_w_out, ev_ffn_norm, ev_w_gate, ev_w_up, ev_w_down,
              od_attn_norm, od_w_qkv, od_q_norm, od_k_norm, od_rel_bias, od_w_out, od_ffn_norm, od_router,
              od_we_gate, od_we_up, od_we_down):
    cos, sin = rope_tables(positions)
    for layer in range(DEPTH):
        i = layer // 2
        if layer % 2 == 0:
            x = x + even_mixer(rms_norm(x, ev_attn_norm[i]), cos, sin, ev_w_in[i], ev_q_norm[i], ev_k_norm[i],
                               ev_lambda_q1[i], ev_lambda_k1[i], ev_lambda_q2[i], ev_lambda_k2[i],
                               ev_subln[i], ev_w_out[i], layer)
            x = x + swiglu(rms_norm(x, ev_ffn_norm[i]), ev_w_gate[i], ev_w_up[i], ev_w_down[i])
        else:
            x = x + odd_mixer(rms_norm(x, od_attn_norm[i]), od_w_qkv[i], od_q_norm[i], od_k_norm[i],
                              od_rel_bias[i], od_w_out[i])
            x = x + moe_swiglu(rms_norm(x, od_ffn_norm[i]), od_router[i], od_we_gate[i], od_we_up[i],
                               od_we_down[i])
    return x
```

```python
import contextlib
import numpy as np
import ml_dtypes
import concourse.bass as bass
import concourse.mybir as mybir
from concourse.bass_utils import run_bass_kernel_spmd

F32 = mybir.dt.float32
BF16 = mybir.dt.bfloat16
I32 = mybir.dt.int32
AF = mybir.ActivationFunctionType
ALU = mybir.AluOpType
AX = mybir.AxisListType

D = 1024
KC = 8
EPS = 1e-6
NCORES = 8

ENGS = ("pe", "act", "dve", "pool", "sp")


class Buf:
    __slots__ = ("name", "last_w", "readers", "excl")

    def __init__(self, name, excl=False):
        self.name = name
        self.last_w = None
        self.readers = []
        self.excl = excl


class Op:
    __slots__ = ("eng", "fn", "deps", "is_dma", "stream", "ndma", "signal", "val", "sem", "seq")

    def __init__(self, eng, fn):
        self.eng = eng
        self.fn = fn
        self.deps = []
        self.is_dma = False
        self.stream = None
        self.ndma = 0
        self.signal = False
        self.val = 0
        self.sem = None
        self.seq = 0


class Prog:
    def __init__(self, nc):
        self.nc = nc
        self.ops = {e: [] for e in ENGS}
        self.all_ops = []
        self.streams = {}
        self._fence_pos = 0

    def buf(self, name, excl=False):
        return Buf(name, excl)

    def _deps(self, op, reads, writes):
        op.seq = len(self.all_ops)
        deps = []
        for b in reads:
            if b.last_w is not None:
                deps.append(b.last_w)
            if b.excl:
                deps.extend(r for r in b.readers if r.eng != op.eng)
        for b in writes:
            if b.last_w is not None:
                deps.append(b.last_w)
            deps.extend(b.readers)
        best = {}
        for d in deps:
            if d is op:
                continue
            if d.is_dma:
                key = ("dma", d.stream)
            else:
                if d.eng == "pe" and op.eng == "pe" and not op.is_dma:
                    continue
                key = d.eng
            if key not in best or best[key].seq < d.seq:
                best[key] = d
        for d in best.values():
            op.deps.append(d)
            d.signal = True
        for b in reads:
            b.readers = [r for r in b.readers if r.is_dma or r.eng != op.eng or op.is_dma] + [op]
        for b in writes:
            b.last_w = op
            b.readers = []

    def op(self, eng, fn, reads=(), writes=()):
        o = Op(eng, fn)
        self._deps(o, reads, writes)
        self.ops[eng].append(o)
        self.all_ops.append(o)
        return o

    def dma(self, eng, fn, stream, ndma=1, reads=(), writes=()):
        o = Op(eng, fn)
        o.is_dma = True
        o.stream = stream
        o.ndma = ndma
        self._deps(o, reads, writes)
        self.ops[eng].append(o)
        self.all_ops.append(o)
        o.signal = True
        return o

    def wait_all(self, eng, bufs):
        o = Op(eng, None)
        self._deps(o, (), bufs)
        self.ops[eng].append(o)
        self.all_ops.append(o)
        return o

    def fence(self):
        last = []
        for e in ENGS:
            for o in reversed(self.ops[e]):
                if not o.is_dma and o.fn is not None:
                    last.append(o)
                    break
        dmas = [o for o in self.all_ops[self._fence_pos:] if o.is_dma]
        self._fence_pos = len(self.all_ops)
        for e in ENGS:
            o = Op(e, None)
            o.seq = len(self.all_ops)
            for d in last + dmas:
                if d.eng == e and not d.is_dma:
                    continue
                o.deps.append(d)
                d.signal = True
            self.ops[e].append(o)
            self.all_ops.append(o)

    def emit(self, stack):
        nc = self.nc
        eng_sem = {}
        for e in ENGS:
            eng_sem[e] = stack.enter_context(nc.semaphore("s_" + e))
        stream_sem = {}
        stream_cnt = {}
        cnt = {e: 0 for e in ENGS}
        for o in self.all_ops:
            if o.is_dma:
                if o.stream not in stream_sem:
                    stream_sem[o.stream] = stack.enter_context(
                        nc.semaphore("d_%d" % len(stream_sem)))
                    stream_cnt[o.stream] = 0
                stream_cnt[o.stream] += 16 * o.ndma
                o.sem = stream_sem[o.stream]
                o.val = stream_cnt[o.stream]
            elif o.signal:
                cnt[o.eng] += 1
                o.sem = eng_sem[o.eng]
                o.val = cnt[o.eng]
        self.nsem = len(stream_sem) + len(ENGS)
        self.sigcounts = dict(cnt)
        block = stack.enter_context(nc.Block())
        hw = {"pe": block.tensor, "act": block.scalar, "dve": block.vector,
              "pool": block.gpsimd, "sp": block.sync}
        for e in ENGS:
            ops = self.ops[e]
            if not ops:
                continue

            def body(engine, ops=ops):
                known = {}
                for o in ops:
                    need = {}
                    for d in o.deps:
                        key = id(d.sem)
                        if key not in need or need[key][1] < d.val:
                            need[key] = (d.sem, d.val)
                    for key, (sem, val) in need.items():
                        if known.get(key, 0) >= val:
                            continue
                        engine.wait_ge(sem, val)
                        known[key] = val
                    if o.fn is None:
                        continue
                    r = o.fn(engine)
                    if o.is_dma:
                        assert len(r) == o.ndma, (len(r), o.ndma)
                        for inst in r:
                            inst.then_inc(o.sem, 16)
                    elif o.signal:
                        r.then_inc(o.sem, 1)

            hw[e](body)


def ffn_blocks(dff):
    nfc = dff // 128
    blocks = []
    c = 0
    while c < nfc:
        n = min(4, nfc - c)
        blocks.append((c, n))
        c += n
    return blocks


def emit_ffn(P, nc, st, xres, xres_b, xnT, xnT_b, jobs, ps, ps_b, ntile, tag="f"):
    ngrp = ntile // 4
    wgb, wub, wdb, wb_b = [], [], [], []
    for i in range(2):
        wgb.append(st.enter_context(nc.sbuf_tensor("%s_wg%d" % (tag, i), [128, KC, 512], BF16)))
        wub.append(st.enter_context(nc.sbuf_tensor("%s_wu%d" % (tag, i), [128, KC, 512], BF16)))
        wdb.append(st.enter_context(nc.sbuf_tensor("%s_wd%d" % (tag, i), [128, 4, D], BF16)))
        wb_b.append(P.buf("%s_w%d" % (tag, i)))
    hT, hT_b, sg, sg_b = [], [], [], []
    for i in range(2):
        hT.append(st.enter_context(nc.sbuf_tensor("%s_hT%d" % (tag, i), [128, 4, 512], BF16)))
        hT_b.append(P.buf("%s_hT%d" % (tag, i)))
        sg.append(st.enter_context(nc.sbuf_tensor("%s_sg%d" % (tag, i), [128, 512], F32)))
        sg_b.append(P.buf("%s_sg%d" % (tag, i)))

    def load_w(ji, slot):
        wg_d, wu_d, wd_d, c0, n, _, _ = jobs[ji]
        wg_v = wg_d.rearrange("(kc p) f -> p kc f", p=128)
        wu_v = wu_d.rearrange("(kc p) f -> p kc f", p=128)
        wd_v = wd_d.rearrange("(fc p) d -> p fc d", p=128)

        def fn(e):
            r = []
            r.append(e.dma_start(out=wgb[slot][:, :, 0:n * 128], in_=wg_v[:, :, c0 * 128:(c0 + n) * 128]))
            r.append(e.dma_start(out=wub[slot][:, :, 0:n * 128], in_=wu_v[:, :, c0 * 128:(c0 + n) * 128]))
            r.append(e.dma_start(out=wdb[slot][:, 0:n, :], in_=wd_v[:, c0:c0 + n, :]))
            return r
        P.dma("pool", fn, stream=("w", tag, slot), ndma=3, writes=[wb_b[slot]])

    load_w(0, 0)
    it = 0
    for ji, (_, _, _, c0, n, comb, comb_b) in enumerate(jobs):
        slot = ji % 2
        if ji + 1 < len(jobs):
            load_w(ji + 1, (ji + 1) % 2)
        for g in range(ngrp):
            hs = it % 2
            it += 1
            tok = slice(g * 512, (g + 1) * 512)
            for j in range(n):
                pg, pu = ps[(2 * j) % 4], ps[(2 * j + 1) % 4]
                pgb, pub = ps_b[(2 * j) % 4], ps_b[(2 * j + 1) % 4]
                for kc in range(KC):
                    P.op("pe", lambda e, pg=pg, slot=slot, kc=kc, j=j, tok=tok: e.matmul(
                        pg[:], lhsT=wgb[slot][:, kc, j * 128:(j + 1) * 128], rhs=xnT[:, kc, tok],
                        start=(kc == 0), stop=(kc == KC - 1)), reads=[wb_b[slot], xnT_b], writes=[pgb])
                for kc in range(KC):
                    P.op("pe", lambda e, pu=pu, slot=slot, kc=kc, j=j, tok=tok: e.matmul(
                        pu[:], lhsT=wub[slot][:, kc, j * 128:(j + 1) * 128], rhs=xnT[:, kc, tok],
                        start=(kc == 0), stop=(kc == KC - 1)), reads=[wb_b[slot], xnT_b], writes=[pub])
                ss = j % 2
                P.op("act", lambda e, pg=pg, ss=ss: e.activation(out=sg[ss][:], in_=pg[:], func=AF.Silu),
                     reads=[pgb], writes=[sg_b[ss]])
                P.op("dve", lambda e, pu=pu, ss=ss, hs=hs, j=j: e.tensor_tensor(
                    out=hT[hs][:, j, :], in0=pu[:], in1=sg[ss][:], op=ALU.mult),
                    reads=[pub, sg_b[ss]], writes=[hT_b[hs]])
            for tt in range(4):
                t = g * 4 + tt
                for half in range(2):
                    pi = 4 + ((tt * 2 + half) % 4)
                    po, pob = ps[pi], ps_b[pi]
                    for j in range(n):
                        P.op("pe", lambda e, po=po, hs=hs, j=j, tt=tt, slot=slot, half=half, n=n: e.matmul(
                            po[:], lhsT=hT[hs][:, j, tt * 128:(tt + 1) * 128],
                            rhs=wdb[slot][:, j, half * 512:(half + 1) * 512],
                            start=(j == 0), stop=(j == n - 1)), reads=[hT_b[hs], wb_b[slot]], writes=[pob])
                    xs = xres[:, t, half * 512:(half + 1) * 512]
                    if comb is None:
                        P.op("dve", lambda e, po=po, xs=xs: e.tensor_tensor(out=xs, in0=po[:], in1=xs, op=ALU.add),
                             reads=[pob, xres_b[t]], writes=[xres_b[t]])
                    else:
                        cs = comb(t)
                        P.op("dve", lambda e, po=po, xs=xs, cs=cs: e.scalar_tensor_tensor(
                            out=xs, in0=po[:], scalar=cs, in1=xs, op0=ALU.mult, op1=ALU.add),
                            reads=[pob, xres_b[t], comb_b], writes=[xres_b[t]])


def emit_rstd(P, ss_ap, rstd_ap, ss_b, rstd_b, inv_n, eps_ap):
    P.op("act", lambda e: e.activation(out=rstd_ap, in_=ss_ap, func=AF.Ln, bias=eps_ap, scale=inv_n),
         reads=[ss_b], writes=[rstd_b])
    P.op("act", lambda e: e.activation(out=rstd_ap, in_=rstd_ap, func=AF.Exp, scale=-0.5),
         reads=[rstd_b], writes=[rstd_b])


class NormCtx:
    def __init__(self, P, nc, st, tag="n"):
        self.junk = st.enter_context(nc.sbuf_tensor(tag + "_junk", [128, D], BF16))
        self.junk_b = P.buf(tag + "_junk")
        self.xb = [st.enter_context(nc.sbuf_tensor(tag + "_xb%d" % i, [128, D], BF16)) for i in range(2)]
        self.xb_b = [P.buf(tag + "_xb%d" % i) for i in range(2)]
        self.ss = [st.enter_context(nc.sbuf_tensor(tag + "_ss%d" % i, [128, 2], F32)) for i in range(2)]
        self.ss_b = [P.buf(tag + "_ss%d" % i) for i in range(2)]
        self.eps = st.enter_context(nc.sbuf_tensor(tag + "_eps", [128, 1], F32))
        self.eps_b = P.buf(tag + "_eps")
        P.op("pool", lambda e: e.memset(self.eps[:], EPS), writes=[self.eps_b])
        self.i = 0


def emit_norm_tile(P, N, xt, xt_b, g_bc, g_b, psT, psT_b, ident, ident_b, out_ap, out_b, xn32=None, xn32_b=None):
    s = N.i % 2
    N.i += 1
    ss, ss_b = N.ss[s], N.ss_b[s]
    P.op("act", lambda e: e.activation(out=N.junk[:], in_=xt, func=AF.Square, accum_out=ss[:, 0:1]),
         reads=[xt_b], writes=[N.junk_b, ss_b])
    emit_rstd(P, ss[:, 0:1], ss[:, 1:2], ss_b, ss_b, 1.0 / D, N.eps[:, 0:1])
    if xn32 is not None:
        P.op("dve", lambda e: e.scalar_tensor_tensor(out=xn32, in0=xt, scalar=ss[:, 1:2], in1=g_bc,
                                                     op0=ALU.mult, op1=ALU.mult),
             reads=[xt_b, ss_b, g_b], writes=[xn32_b])
        P.op("pool", lambda e: e.tensor_copy(out=N.xb[s][:], in_=xn32), reads=[xn32_b], writes=[N.xb_b[s]])
    else:
        P.op("dve", lambda e: e.scalar_tensor_tensor(out=N.xb[s][:], in0=xt, scalar=ss[:, 1:2], in1=g_bc,
                                                     op0=ALU.mult, op1=ALU.mult),
             reads=[xt_b, ss_b, g_b], writes=[N.xb_b[s]])
    emit_transpose8(P, N.xb[s], N.xb_b[s], psT, psT_b, ident, ident_b, out_ap, out_b)


def emit_transpose8(P, src, src_b, psT, psT_b, ident, ident_b, out_ap, out_b, eng="act"):
    for kc in range(KC):
        P.op("pe", lambda e, kc=kc: e.transpose(out=psT[:, kc * 128:(kc + 1) * 128],
                                                in_=src[:, kc * 128:(kc + 1) * 128], identity=ident[:]),
             reads=[src_b, ident_b], writes=[psT_b])
    pv = psT.rearrange("p (k t) -> p k t", k=KC)
    if eng == "act":
        P.op("act", lambda e: e.activation(out=out_ap, in_=pv, func=AF.Copy), reads=[psT_b], writes=[out_b])
    else:
        P.op("dve", lambda e: e.tensor_copy(out=out_ap, in_=pv), reads=[psT_b], writes=[out_b])


def alloc_psum(P, nc, st):
    ps = [st.enter_context(nc.psum_tensor("ps%d" % i, [128, 512], F32)) for i in range(8)]
    ps_b = [P.buf("ps%d" % i, excl=True) for i in range(8)]
    return ps, ps_b


def make_ident(P, nc, st):
    identf = st.enter_context(nc.sbuf_tensor("identf", [128, 128], F32))
    ident = st.enter_context(nc.sbuf_tensor("ident", [128, 128], BF16))
    b = P.buf("ident")
    P.op("pool", lambda e: e.memset(identf[:], 1.0), writes=[b])
    P.op("pool", lambda e: e.affine_select(out=identf[:], in_=identf[:], pattern=[[-1, 128]],
                                           compare_op=ALU.is_equal, fill=0.0, base=0, channel_multiplier=1),
         reads=[b], writes=[b])
    P.op("pool", lambda e: e.tensor_copy(out=ident[:], in_=identf[:]), reads=[b], writes=[b])
    return ident, b, identf


def build_b1(ntile=16):
    nc = bass.Bass("TRN2", target_bir_lowering=False)
    T = ntile * 128
    DFF = 2816
    x_d = nc.dram_tensor("x", [T, D], F32, kind="ExternalInput").ap()
    mT_d = nc.dram_tensor("mT", [D, T], BF16, kind="ExternalInput").ap()
    wo_d = nc.dram_tensor("w_out", [D, D], F32, kind="ExternalInput").ap()
    g_d = nc.dram_tensor("g", [D], F32, kind="ExternalInput").ap()
    wg_d = nc.dram_tensor("w_gate", [D, DFF], F32, kind="ExternalInput").ap()
    wu_d = nc.dram_tensor("w_up", [D, DFF], F32, kind="ExternalInput").ap()
    wd_d = nc.dram_tensor("w_down", [DFF, D], F32, kind="ExternalInput").ap()
    y_d = nc.dram_tensor("y", [T, D], F32, kind="ExternalOutput").ap()
    P = Prog(nc)
    with contextlib.ExitStack() as st:
        ps, ps_b = alloc_psum(P, nc, st)
        ident, ident_b, identf = make_ident(P, nc, st)
        xres = st.enter_context(nc.sbuf_tensor("xres", [128, ntile, D], F32))
        xres_b = [P.buf("xres%d" % t) for t in range(ntile)]
        mT = st.enter_context(nc.sbuf_tensor("mTs", [128, KC, T], BF16))
        mT_b = P.buf("mT")
        wo = st.enter_context(nc.sbuf_tensor("wo", [128, KC, D], BF16))
        wo_b = P.buf("wo")
        g_bc = st.enter_context(nc.sbuf_tensor("g_bc", [128, D], F32))
        g_b = P.buf("g")
        xnT, xnT_b = mT, mT_b
        x_v = x_d.rearrange("(t p) d -> p t d", p=128)
        for t in range(ntile):
            P.dma("sp", lambda e, t=t: [e.dma_start(out=xres[:, t, :], in_=x_v[:, t, :])],
                  stream=("x", t), writes=[xres_b[t]])
        P.dma("sp", lambda e: [e.dma_start(out=mT[:], in_=mT_d.rearrange("(kc p) t -> p kc t", p=128))],
              stream="mT", writes=[mT_b])
        P.dma("pool", lambda e: [e.dma_start(out=wo[:], in_=wo_d.rearrange("(kc p) d -> p kc d", p=128))],
              stream="wo", writes=[wo_b])
        P.dma("sp", lambda e: [e.dma_start(out=g_bc[:], in_=g_d.partition_broadcast(128))],
              stream="g", writes=[g_b])
        for t in range(ntile):
            for half in range(2):
                pi = (t * 2 + half) % 8
                for kc in range(KC):
                    P.op("pe", lambda e, pi=pi, kc=kc, t=t, half=half: e.matmul(
                        ps[pi][:], lhsT=mT[:, kc, t * 128:(t + 1) * 128], rhs=wo[:, kc, half * 512:(half + 1) * 512],
                        start=(kc == 0), stop=(kc == KC - 1)), reads=[mT_b, wo_b], writes=[ps_b[pi]])
                xs = xres[:, t, half * 512:(half + 1) * 512]
                P.op("dve", lambda e, pi=pi, xs=xs: e.tensor_tensor(out=xs, in0=ps[pi][:], in1=xs, op=ALU.add),
                     reads=[ps_b[pi], xres_b[t]], writes=[xres_b[t]])
        N = NormCtx(P, nc, st)
        psT = ps[0][:].bitcast(BF16)
        for t in range(ntile):
            emit_norm_tile(P, N, xres[:, t, :], xres_b[t], g_bc[:], g_b, psT, ps_b[0], ident, ident_b,
                           xnT[:, :, t * 128:(t + 1) * 128], xnT_b)
        jobs = [(wg_d, wu_d, wd_d, c0, n, None, None) for (c0, n) in ffn_blocks(DFF)]
        emit_ffn(P, nc, st, xres, xres_b, xnT, xnT_b, jobs, ps, ps_b, ntile)
        y_v = y_d.rearrange("(t p) d -> p t d", p=128)
        for t in range(ntile):
            P.dma("sp", lambda e, t=t: [e.dma_start(out=y_v[:, t, :], in_=xres[:, t, :])],
                  stream=("x", t), reads=[xres_b[t]])
        P.wait_all("sp", xres_b)
        P.emit(st)
    return nc


NEG = -30000.0
NH1 = 16
DFFE = 3584
NEXP = 8


def band_bias_index():
    kb = np.arange(5)[:, None, None]
    j = np.arange(128)[None, :, None]
    i = np.arange(128)[None, None, :]
    ck = 2 * kb + j // 64
    cq = i // 64
    jband = (ck - cq) * 64 + j % 64
    rel = np.clip(512 + (i % 64) - jband, -128, 128) + 128
    valid = (ck - cq >= 0) & (ck - cq <= 8)
    rel = np.where(valid, rel, 0)
    return rel.astype(np.int64)


def emit_l1_attn(P, nc, st, ps, ps_b, ident, ident_b, identf, xres, xres_b, xh_d, halo_d, ga_d, wqkv_d, gq_d, gk_d,
                bm_d, cb_d, wo_d, ntile=16):
    T = ntile * 128
    with contextlib.ExitStack() as sa:
        N = NormCtx(P, nc, sa, tag="a")
        xh = sa.enter_context(nc.sbuf_tensor("xh_sb", [128, 2, D], F32))
        xh_b = [P.buf("xh%d" % t) for t in range(2)]
        xh_v = xh_d.rearrange("(t p) d -> p t d", p=128)
        halo = sa.enter_context(nc.sbuf_tensor("halo_sb", [128, 1], F32))
        halo_b = P.buf("halo")
        P.dma("sp", lambda e: [e.dma_start(out=halo[:], in_=halo_d)], stream="halo", writes=[halo_b])
        ga = sa.enter_context(nc.sbuf_tensor("ga_sb", [128, D], F32))
        gq = sa.enter_context(nc.sbuf_tensor("gq_sb", [128, 512], F32))
        gk = sa.enter_context(nc.sbuf_tensor("gk_sb", [128, 512], F32))
        gv_b = P.buf("gains")
        P.dma("sp", lambda e: [e.dma_start(out=ga[:], in_=ga_d.partition_broadcast(128)),
                               e.dma_start(out=gq[:], in_=gq_d.partition_broadcast(128)),
                               e.dma_start(out=gk[:], in_=gk_d.partition_broadcast(128))],
              stream="gains", ndma=3, writes=[gv_b])
        P.op("pool", lambda e: e.tensor_scalar(out=gq[:], in0=gq[:], scalar1=0.125, scalar2=1.0,
                                               op0=ALU.mult, op1=ALU.mult), reads=[gv_b], writes=[gv_b])
        wqkv = sa.enter_context(nc.sbuf_tensor("wqkv_sb", [128, KC, 3 * D], BF16))
        wqkv_b = P.buf("wqkv")
        wv = wqkv_d.rearrange("(kc p) f -> p kc f", p=128)
        P.dma("pool", lambda e: [e.dma_start(out=wqkv[:, :, c3 * D:(c3 + 1) * D], in_=wv[:, :, c3 * D:(c3 + 1) * D])
                                 for c3 in range(3)], stream="wqkv", ndma=3, writes=[wqkv_b])
        wo = sa.enter_context(nc.sbuf_tensor("wo1", [128, KC, D], BF16))
        wo_b = P.buf("wo1")
        P.dma("pool", lambda e: [e.dma_start(out=wo[:], in_=wo_d.rearrange("(kc p) d -> p kc d", p=128))],
              stream="wo1", writes=[wo_b])
        bm = sa.enter_context(nc.sbuf_tensor("bm_sb", [128, NH1, 2, 128], BF16))
        bm_b = P.buf("bm")
        P.dma("pool", lambda e: [e.dma_start(out=bm[:], in_=bm_d, max_dma_last_dim=4096)], stream="bm", writes=[bm_b])
        P.op("pool", lambda e: e.memset(bm[64:128, :, 1, 0:64], NEG), reads=[bm_b], writes=[bm_b])
        m0 = sa.enter_context(nc.sbuf_tensor("m0_sb", [128, 128], BF16))
        P.op("pool", lambda e: e.memset(m0[:], 0.0), writes=[bm_b])
        P.op("pool", lambda e: e.memset(m0[0:64, 64:128], NEG), reads=[bm_b], writes=[bm_b])
        cb = sa.enter_context(nc.sbuf_tensor("cb_sb", [128, 2, NH1], F32))
        cb_b = P.buf("cb")
        P.dma("sp", lambda e: [e.dma_start(out=cb[:, 0, :], in_=cb_d.partition_broadcast(128))], stream="cb",
              writes=[cb_b])
        P.op("dve", lambda e: e.tensor_scalar(out=cb[:, 1, :], in0=cb[:, 0, :], scalar1=halo[:, 0:1], scalar2=None,
                                              op0=ALU.add), reads=[cb_b, halo_b], writes=[cb_b])
        NR = 6
        kT_r = [sa.enter_context(nc.sbuf_tensor("kTr%d" % i, [128, KC, 128], BF16)) for i in range(NR)]
        kT_rb = [P.buf("kTr%d" % i) for i in range(NR)]
        v_r = [sa.enter_context(nc.sbuf_tensor("vr%d" % i, [128, NH1, 65], BF16)) for i in range(NR)]
        v_rb = [P.buf("vr%d" % i) for i in range(NR)]
        for i in range(NR):
            P.op("pool", lambda e, i=i: e.memset(v_r[i][:, :, 64:65], 1.0), writes=[v_rb[i]])
        hT = sa.enter_context(nc.sbuf_tensor("hT1", [128, KC, 128], BF16))
        hT_b = P.buf("hT1")
        qT = sa.enter_context(nc.sbuf_tensor("qT1", [128, KC, 128], BF16))
        qT_b = P.buf("qT1")
        sq = sa.enter_context(nc.sbuf_tensor("sq1", [128, D], F32))
        sq_b = P.buf("sq1")
        ssq = sa.enter_context(nc.sbuf_tensor("ssq1", [128, 32], F32))
        ssq_b = P.buf("ssq1")
        tmp, tmp_b = sq, sq_b
        qkn = sa.enter_context(nc.sbuf_tensor("qkn1", [128, D], BF16))
        qkn_b = P.buf("qkn1")
        wsb = [sa.enter_context(nc.sbuf_tensor("w1_%d" % i, [128, 5, 128], BF16)) for i in range(2)]
        wsb_b = [P.buf("w1_%d" % i) for i in range(2)]
        osb = sa.enter_context(nc.sbuf_tensor("o1", [128, D], BF16))
        osb_b = P.buf("o1")
        oT = sa.enter_context(nc.sbuf_tensor("oT1", [128, KC, 128], BF16))
        oT_b = P.buf("oT1")
        rden = sa.enter_context(nc.sbuf_tensor("rden1", [128, NH1], F32))
        rden_b = P.buf("rden1")
        psT = ps[1][:].bitcast(BF16)
        psT_b = ps_b[1]

        def qk_norm(pa, pa_b, pb, pb_b, g_bc, dst, dst_b, col):
            for hf, (pp, ppb) in enumerate(((pa, pa_b), (pb, pb_b))):
                P.op("act", lambda e, pp=pp, hf=hf: e.activation(out=sq[:, hf * 512:(hf + 1) * 512], in_=pp[:],
                                                                 func=AF.Square), reads=[ppb], writes=[sq_b])
            P.op("dve", lambda e: e.tensor_reduce(out=ssq[:, col:col + 16],
                                                  in_=sq[:].rearrange("p (h d) -> p h d", d=64),
                                                  axis=AX.X, op=ALU.add), reads=[sq_b], writes=[ssq_b])
            emit_rstd(P, ssq[:, col:col + 16], ssq[:, col:col + 16], ssq_b, ssq_b, 1.0 / 64, N.eps[:, 0:1])
            for hf, (pp, ppb) in enumerate(((pa, pa_b), (pb, pb_b))):
                P.op("dve", lambda e, pp=pp, hf=hf: e.tensor_tensor(
                    out=tmp[:, hf * 512:(hf + 1) * 512].rearrange("p (h d) -> p h d", d=64),
                    in0=pp[:].rearrange("p (h d) -> p h d", d=64),
                    in1=ssq[:, col + hf * 8:col + hf * 8 + 8].unsqueeze(2).to_broadcast([128, 8, 64]),
                    op=ALU.mult), reads=[ppb, ssq_b], writes=[tmp_b])
                P.op("pool", lambda e, hf=hf: e.tensor_tensor(
                    out=qkn[:, hf * 512:(hf + 1) * 512], in0=tmp[:, hf * 512:(hf + 1) * 512], in1=g_bc[:],
                    op=ALU.mult), reads=[tmp_b, gv_b], writes=[qkn_b])
            emit_transpose8(P, qkn, qkn_b, psT, psT_b, ident, ident_b, dst[:], dst_b, eng="dve")

        for lb in range(ntile + 4):
            own = lb >= 4
            m = lb - 4
            r = lb % NR
            if own:
                xt, xt_b = xres[:, m, :], xres_b[m]
            else:
                P.dma("sp", lambda e, lb=lb: [e.dma_start(out=xh[:, lb % 2, :], in_=xh_v[:, lb, :])],
                      stream=("xh", lb % 2), writes=[xh_b[lb % 2]])
                xt, xt_b = xh[:, lb % 2, :], xh_b[lb % 2]
            emit_norm_tile(P, N, xt, xt_b, ga[:], gv_b, ps[0][:].bitcast(BF16), ps_b[0], ident, ident_b, hT[:], hT_b)
            banks = ([(2, 0), (3, 512)] if own else []) + [(4, D), (5, D + 512), (6, 2 * D), (7, 2 * D + 512)]
            for pi, c0 in banks:
                for kc in range(KC):
                    P.op("pe", lambda e, pi=pi, c0=c0, kc=kc: e.matmul(
                        ps[pi][:], lhsT=hT[:, kc, :], rhs=wqkv[:, kc, c0:c0 + 512],
                        start=(kc == 0), stop=(kc == KC - 1)), reads=[hT_b, wqkv_b], writes=[ps_b[pi]])
            qk_norm(ps[4], ps_b[4], ps[5], ps_b[5], gk, kT_r[r], kT_rb[r], 16)
            for hf in range(2):
                P.op("act", lambda e, hf=hf, r=r: e.activation(
                    out=v_r[r][:, hf * 8:(hf + 1) * 8, 0:64],
                    in_=ps[6 + hf][:].rearrange("p (h d) -> p h d", d=64), func=AF.Copy),
                    reads=[ps_b[6 + hf]], writes=[v_rb[r]])
            if not own:
                continue
            qk_norm(ps[2], ps_b[2], ps[3], ps_b[3], gq, qT, qT_b, 0)
            obank = [(6, 0)] * 7 + [(7, 0)] * 7 + [(0, 0)] * 2
            for h in range(NH1):
                hp, po = h // 2, 64 * (h % 2)
                sset = h % 2
                S0, S1 = ps[2 + 2 * sset], ps[3 + 2 * sset]
                S0b, S1b = ps_b[2 + 2 * sset], ps_b[3 + 2 * sset]
                for kb in range(5):
                    rk = (m + kb) % NR
                    dst, dstb = (S0[:, kb * 128:(kb + 1) * 128], S0b) if kb < 4 else (S1[:, 0:128], S1b)
                    extra = kb in (0, 3, 4)
                    P.op("pe", lambda e, dst=dst, rk=rk, po=po, hp=hp, extra=extra: e.matmul(
                        dst, lhsT=kT_r[rk][po:po + 64, hp, :], rhs=qT[po:po + 64, hp, :], start=True,
                        stop=not extra), reads=[kT_rb[rk], qT_b], writes=[dstb])
                    if extra:
                        rhs = m0[:] if kb == 0 else bm[:, h, kb - 3, :]
                        P.op("pe", lambda e, dst=dst, rhs=rhs: e.matmul(dst, lhsT=ident[:], rhs=rhs, start=False,
                                                                        stop=True),
                             reads=[ident_b, bm_b], writes=[dstb])
                ws = h % 2
                nh = max(0, min(4, 4 - m))
                segs = []
                if min(nh, 3) > 0:
                    segs.append((0, min(nh, 3), cb[:, 1, h:h + 1]))
                if min(nh, 3) < 3:
                    segs.append((min(nh, 3), 3, cb[:, 0, h:h + 1]))
                segs.append((3, 4, halo[:, 0:1] if nh == 4 else None))
                for (a, b_, bias) in segs:
                    if bias is None:
                        P.op("act", lambda e, a=a, b_=b_, ws=ws, S0=S0: e.activation(
                            out=wsb[ws][:, a:b_, :], in_=S0[:, a * 128:b_ * 128].rearrange("p (k q) -> p k q", q=128),
                            func=AF.Exp), reads=[S0b], writes=[wsb_b[ws]])
                    else:
                        P.op("act", lambda e, a=a, b_=b_, ws=ws, S0=S0, bias=bias: e.activation(
                            out=wsb[ws][:, a:b_, :], in_=S0[:, a * 128:b_ * 128].rearrange("p (k q) -> p k q", q=128),
                            func=AF.Exp, bias=bias), reads=[S0b, halo_b, cb_b], writes=[wsb_b[ws]])
                P.op("act", lambda e, ws=ws, S1=S1: e.activation(out=wsb[ws][:, 4, :], in_=S1[:, 0:128], func=AF.Exp),
                     reads=[S1b], writes=[wsb_b[ws]])
                ob = obank[h][0]
                oc = (h % 7 if h < 14 else h - 14) * 65
                for kb in range(5):
                    rk = (m + kb) % NR
                    P.op("pe", lambda e, ob=ob, oc=oc, ws=ws, kb=kb, rk=rk, h=h: e.matmul(
                        ps[ob][:, oc:oc + 65], lhsT=wsb[ws][:, kb, :], rhs=v_r[rk][:, h, :],
                        start=(kb == 0), stop=(kb == 4)), reads=[wsb_b[ws], v_rb[rk]], writes=[ps_b[ob]])
            for ob, h0, nh_ in ((6, 0, 7), (7, 7, 7), (0, 14, 2)):
                pv = ps[ob][:, 0:nh_ * 65].rearrange("p (h d) -> p h d", d=65)
                P.op("dve", lambda e, pv=pv, h0=h0, nh_=nh_: e.reciprocal(
                    out=rden[:, h0:h0 + nh_].unsqueeze(2), in_=pv[:, :, 64:65]), reads=[ps_b[ob]], writes=[rden_b])
                P.op("dve", lambda e, pv=pv, h0=h0, nh_=nh_: e.tensor_tensor(
                    out=osb[:, h0 * 64:(h0 + nh_) * 64].rearrange("p (h d) -> p h d", d=64), in0=pv[:, :, 0:64],
                    in1=rden[:, h0:h0 + nh_].unsqueeze(2).to_broadcast([128, nh_, 64]), op=ALU.mult),
                    reads=[ps_b[ob], rden_b], writes=[osb_b])
            emit_transpose8(P, osb, osb_b, psT, psT_b, ident, ident_b, oT[:], oT_b, eng="act")
            for half in range(2):
                pi = 4 + half
                for kc in range(KC):
                    P.op("pe", lambda e, pi=pi, kc=kc, half=half: e.matmul(
                        ps[pi][:], lhsT=oT[:, kc, :], rhs=wo[:, kc, half * 512:(half + 1) * 512],
                        start=(kc == 0), stop=(kc == KC - 1)), reads=[oT_b, wo_b], writes=[ps_b[pi]])
                xs = xres[:, m, half * 512:(half + 1) * 512]
                P.op("dve", lambda e, pi=pi, xs=xs: e.tensor_tensor(out=xs, in0=ps[pi][:], in1=xs, op=ALU.add),
                     reads=[ps_b[pi], xres_b[m]], writes=[xres_b[m]])
        P.fence()


def emit_l1_moe(P, nc, st, ps, ps_b, ident, ident_b, identf, xres, xres_b, gf_d, wr_d, weg_d, weu_d, wed_d,
                ntile=16, nexp_run=NEXP, stage=9):
    T = ntile * 128
    with contextlib.ExitStack() as sm:
        N = NormCtx(P, nc, sm, tag="m")
        gf = sm.enter_context(nc.sbuf_tensor("gf_sb", [128, D], F32))
        gf_b = P.buf("gf")
        P.dma("sp", lambda e: [e.dma_start(out=gf[:], in_=gf_d.partition_broadcast(128))], stream="gf", writes=[gf_b])
        wr = sm.enter_context(nc.sbuf_tensor("wr_sb", [128, KC, NEXP], F32))
        wr_b = P.buf("wr")
        P.dma("sp", lambda e: [e.dma_start(out=wr[:], in_=wr_d.rearrange("(kc p) e -> p kc e", p=128))],
              stream="wr", writes=[wr_b])
        xnT = sm.enter_context(nc.sbuf_tensor("xnT1", [128, KC, T], BF16))
        xnT_b = P.buf("xnT1")
        xn32 = sm.enter_context(nc.sbuf_tensor("xn32", [128, D], F32))
        xn32_b = P.buf("xn32")
        xnT32 = sm.enter_context(nc.sbuf_tensor("xnT32", [128, KC, 128], F32))
        xnT32_b = P.buf("xnT32")
        lg = sm.enter_context(nc.sbuf_tensor("lg", [128, ntile, NEXP], F32))
        lg_b = P.buf("lg")
        for t in range(ntile):
            s_ = N.i % 2
            N.i += 1
            ss, ss_b = N.ss[s_], N.ss_b[s_]
            P.op("act", lambda e, t=t, ss=ss: e.activation(out=N.junk[:], in_=xres[:, t, :], func=AF.Square,
                                                           accum_out=ss[:, 0:1]),
                 reads=[xres_b[t]], writes=[N.junk_b, ss_b])
            emit_rstd(P, ss[:, 0:1], ss[:, 1:2], ss_b, ss_b, 1.0 / D, N.eps[:, 0:1])
            P.op("dve", lambda e, t=t, ss=ss: e.scalar_tensor_tensor(out=xn32[:], in0=xres[:, t, :], scalar=ss[:, 1:2],
                                                                     in1=gf[:], op0=ALU.mult, op1=ALU.mult),
                 reads=[xres_b[t], ss_b, gf_b], writes=[xn32_b])
            for kc in range(KC):
                pi = kc // 4
                P.op("pe", lambda e, kc=kc, pi=pi: e.transpose(
                    out=ps[pi][:, (kc % 4) * 128:(kc % 4 + 1) * 128], in_=xn32[:, kc * 128:(kc + 1) * 128],
                    identity=identf[:]), reads=[xn32_b, ident_b], writes=[ps_b[pi]])
            for pi in range(2):
                pv = ps[pi][:].rearrange("p (k t) -> p k t", k=4)
                P.op("act", lambda e, pi=pi, pv=pv: e.activation(out=xnT32[:, pi * 4:(pi + 1) * 4, :], in_=pv,
                                                                 func=AF.Copy), reads=[ps_b[pi]], writes=[xnT32_b])
                P.op("dve", lambda e, pi=pi, t=t: e.tensor_copy(
                    out=xnT[:, pi * 4:(pi + 1) * 4, t * 128:(t + 1) * 128], in_=xnT32[:, pi * 4:(pi + 1) * 4, :]),
                    reads=[xnT32_b], writes=[xnT_b])
            if stage < 2:
                continue
            for kc in range(KC):
                P.op("pe", lambda e, kc=kc: e.matmul(ps[2][:, 0:NEXP], lhsT=xnT32[:, kc, :], rhs=wr[:, kc, :],
                                                     start=(kc == 0), stop=(kc == KC - 1)),
                     reads=[xnT32_b, wr_b], writes=[ps_b[2]])
            P.op("dve", lambda e, t=t: e.tensor_copy(out=lg[:, t, :], in_=ps[2][:, 0:NEXP]), reads=[ps_b[2]],
                 writes=[lg_b])
        if stage < 3:
            P.fence()
            return
        def small(name, shape):
            return sm.enter_context(nc.sbuf_tensor(name, shape, F32))
        m1 = small("m1", [128, ntile]); m2 = small("m2", [128, ntile])
        k1 = small("k1", [128, ntile, NEXP]); k2 = small("k2", [128, ntile, NEXP]); l2 = small("l2", [128, ntile, NEXP])
        g1 = small("g1", [128, ntile]); g2 = small("g2", [128, ntile]); comb = small("comb", [128, ntile, NEXP])
        rt_b = P.buf("route")
        comb_b = P.buf("comb")
        bc = lambda a: a.unsqueeze(2).to_broadcast([128, ntile, NEXP])
        P.op("dve", lambda e: e.tensor_reduce(out=m1[:], in_=lg[:], axis=AX.X, op=ALU.max), reads=[lg_b], writes=[rt_b])
        P.op("dve", lambda e: e.tensor_tensor(out=k1[:], in0=lg[:], in1=bc(m1[:]), op=ALU.is_equal),
             reads=[lg_b, rt_b], writes=[rt_b])
        P.op("dve", lambda e: e.scalar_tensor_tensor(out=l2[:], in0=k1[:], scalar=-1e30, in1=lg[:],
                                                     op0=ALU.mult, op1=ALU.add), reads=[lg_b, rt_b], writes=[rt_b])
        P.op("dve", lambda e: e.tensor_reduce(out=m2[:], in_=l2[:], axis=AX.X, op=ALU.max), reads=[rt_b], writes=[rt_b])
        P.op("dve", lambda e: e.tensor_tensor(out=k2[:], in0=l2[:], in1=bc(m2[:]), op=ALU.is_equal),
             reads=[rt_b], writes=[rt_b])
        P.op("dve", lambda e: e.tensor_tensor(out=g2[:], in0=m2[:], in1=m1[:], op=ALU.subtract),
             reads=[rt_b], writes=[rt_b])
        P.op("act", lambda e: e.activation(out=g2[:], in_=g2[:], func=AF.Exp), reads=[rt_b], writes=[rt_b])
        P.op("dve", lambda e: e.tensor_scalar(out=g1[:], in0=g2[:], scalar1=1.0, scalar2=None, op0=ALU.add),
             reads=[rt_b], writes=[rt_b])
        P.op("dve", lambda e: e.reciprocal(out=g1[:], in_=g1[:]), reads=[rt_b], writes=[rt_b])
        P.op("dve", lambda e: e.tensor_tensor(out=g2[:], in0=g2[:], in1=g1[:], op=ALU.mult), reads=[rt_b], writes=[rt_b])
        P.op("dve", lambda e: e.tensor_tensor(out=k1[:], in0=k1[:], in1=bc(g1[:]), op=ALU.mult), reads=[rt_b], writes=[rt_b])
        P.op("dve", lambda e: e.tensor_tensor(out=k2[:], in0=k2[:], in1=bc(g2[:]), op=ALU.mult), reads=[rt_b], writes=[rt_b])
        P.op("dve", lambda e: e.tensor_tensor(out=comb[:], in0=k1[:], in1=k2[:], op=ALU.add), reads=[rt_b], writes=[comb_b])
        if stage < 4:
            P.fence()
            return
        jobs = []
        for ex in range(nexp_run):
            for (c0, n) in ffn_blocks(DFFE):
                jobs.append((weg_d[ex], weu_d[ex], wed_d[ex], c0, n,
                             (lambda t, ex=ex: comb[:, t, ex:ex + 1]), comb_b))
        emit_ffn(P, nc, sm, xres, xres_b, xnT, xnT_b, jobs, ps, ps_b, ntile, tag="e")
        P.fence()


def build_b2(ntile=16, do_attn=True, do_moe=True, nexp_run=NEXP, stage=9):
    nc = bass.Bass("TRN2", target_bir_lowering=False)
    T = ntile * 128
    x_d = nc.dram_tensor("x", [T, D], F32, kind="ExternalInput").ap()
    xh_d = nc.dram_tensor("xh", [512, D], F32, kind="ExternalInput").ap()
    halo_d = nc.dram_tensor("halo", [128, 1], F32, kind="ExternalInput").ap()
    ga_d = nc.dram_tensor("ga", [D], F32, kind="ExternalInput").ap()
    wqkv_d = nc.dram_tensor("wqkv", [D, 3 * D], F32, kind="ExternalInput").ap()
    gq_d = nc.dram_tensor("gq", [512], F32, kind="ExternalInput").ap()
    gk_d = nc.dram_tensor("gk", [512], F32, kind="ExternalInput").ap()
    bm_d = nc.dram_tensor("bm", [128, NH1, 2, 128], F32, kind="ExternalInput").ap()
    cb_d = nc.dram_tensor("cb", [NH1], F32, kind="ExternalInput").ap()
    wo_d = nc.dram_tensor("wo", [D, D], F32, kind="ExternalInput").ap()
    gf_d = nc.dram_tensor("gf", [D], F32, kind="ExternalInput").ap()
    wr_d = nc.dram_tensor("wr", [D, NEXP], F32, kind="ExternalInput").ap()
    weg_d = nc.dram_tensor("weg", [nexp_run, D, DFFE], F32, kind="ExternalInput").ap()
    weu_d = nc.dram_tensor("weu", [nexp_run, D, DFFE], F32, kind="ExternalInput").ap()
    wed_d = nc.dram_tensor("wed", [nexp_run, DFFE, D], F32, kind="ExternalInput").ap()
    y_d = nc.dram_tensor("y", [T, D], F32, kind="ExternalOutput").ap()
    P = Prog(nc)
    with contextlib.ExitStack() as st:
        ps, ps_b = alloc_psum(P, nc, st)
        ident, ident_b, identf = make_ident(P, nc, st)
        xres = st.enter_context(nc.sbuf_tensor("xres", [128, ntile, D], F32))
        xres_b = [P.buf("xres%d" % t) for t in range(ntile)]
        x_v = x_d.rearrange("(t p) d -> p t d", p=128)
        for t in range(ntile):
            P.dma("sp", lambda e, t=t: [e.dma_start(out=xres[:, t, :], in_=x_v[:, t, :])],
                  stream=("x", t), writes=[xres_b[t]])
        if do_attn:
            emit_l1_attn(P, nc, st, ps, ps_b, ident, ident_b, identf, xres, xres_b, xh_d, halo_d, ga_d, wqkv_d, gq_d,
                         gk_d, bm_d, cb_d, wo_d, ntile)
        if do_moe:
            emit_l1_moe(P, nc, st, ps, ps_b, ident, ident_b, identf, xres, xres_b, gf_d, wr_d, weg_d, weu_d, wed_d,
                        ntile, nexp_run, stage)
        y_v = y_d.rearrange("(t p) d -> p t d", p=128)
        for t in range(ntile):
            P.dma("sp", lambda e, t=t: [e.dma_start(out=y_v[:, t, :], in_=xres[:, t, :])],
                  stream=("x", t), reads=[xres_b[t]])
        P.wait_all("sp", xres_b)
        P.emit(st)
    return nc


TWO_PI_HI = 6.28125
TWO_PI_LO = 2.0 * np.pi - 6.28125
LAM_INIT0 = 0.8 - 0.6 * 1.0


def emit_l0_attn(P, nc, st, ps, ps_b, ident, ident_b, x_d, pos_d, ga_d, w6_d, gqk_d, lam_d, sub_d, oT_d, NT=64):
    S = NT * 128
    NQB = NT // 4
    sb = lambda name, shape, dt: st.enter_context(nc.sbuf_tensor("a_" + name, shape, dt))
    N = NormCtx(P, nc, st, tag="z")
    qTs = sb("qTs", [128, S], BF16); qTs_b = [P.buf("qTs%d" % g) for g in range(NQB)]
    kTs = sb("kTs", [128, S], BF16); kTs_b = [P.buf("kTs%d" % g) for g in range(NQB)]
    qTd = sb("qTd", [128, S], BF16); qTd_b = [P.buf("qTd%d" % g) for g in range(NQB)]
    kTd = sb("kTd", [128, S], BF16); kTd_b = [P.buf("kTd%d" % g) for g in range(NQB)]
    vs = sb("vs", [128, NT, 128], BF16); vs_b = [P.buf("vs%d" % g) for g in range(NQB)]
    vd = sb("vd", [128, NT, 130], BF16); vd_b = [P.buf("vd%d" % g) for g in range(NQB)]
    osb = sb("osb", [128, NT, 256], BF16); osb_b = [P.buf("osb%d" % g) for g in range(NQB)]
    w6 = sb("w6_sb", [128, KC, 768], BF16); w6_b = P.buf("w6")
    P.dma("pool", lambda e: [e.dma_start(out=w6[:], in_=w6_d.rearrange("(kc p) f -> p kc f", p=128))],
          stream="w6", writes=[w6_b])
    ga = sb("ga0", [128, D], F32); gqk = sb("gqk", [128, 256], F32); subg = sb("subg", [128, 128], F32)
    lam4 = sb("lam4", [128, 4, 64], F32)
    cst_b = P.buf("cst")
    P.dma("sp", lambda e: [e.dma_start(out=ga[:], in_=ga_d.partition_broadcast(128)),
                           e.dma_start(out=gqk[:], in_=gqk_d.partition_broadcast(128)),
                           e.dma_start(out=subg[:], in_=sub_d.partition_broadcast(128)),
                           e.dma_start(out=lam4[:].rearrange("p a b -> p (a b)"), in_=lam_d.partition_broadcast(128))],
          stream="cst", ndma=4, writes=[cst_b])
    P.op("pool", lambda e: e.tensor_scalar(out=gqk[:, 0:128], in0=gqk[:, 0:128], scalar1=0.125, scalar2=1.0,
                                           op0=ALU.mult, op1=ALU.mult), reads=[cst_b], writes=[cst_b])
    P.op("pool", lambda e: e.tensor_scalar(out=subg[:], in0=subg[:], scalar1=1.0 - LAM_INIT0, scalar2=1.0,
                                           op0=ALU.mult, op1=ALU.mult), reads=[cst_b], writes=[cst_b])
    for t in range(NT):
        P.op("pool", lambda e, t=t: e.memset(vd[:, t, 128:130], 1.0), writes=[vd_b[t // 4]])
    lsc = sb("lsc", [128, 4], F32); ljunk = sb("ljunk", [128, 64], F32)
    lam_b = P.buf("lam")
    for i in range(2):
        P.op("dve", lambda e, i=i: e.tensor_tensor(out=ljunk[:], in0=lam4[:, 2 * i, :], in1=lam4[:, 2 * i + 1, :],
                                                   op=ALU.mult), reads=[cst_b], writes=[lam_b])
        P.op("dve", lambda e, i=i: e.tensor_reduce(out=lsc[:, i:i + 1], in_=ljunk[:], axis=AX.X, op=ALU.add),
             reads=[lam_b], writes=[lam_b])
    P.op("act", lambda e: e.activation(out=lsc[:, 0:2], in_=lsc[:, 0:2], func=AF.Exp), reads=[lam_b], writes=[lam_b])
    P.op("dve", lambda e: e.tensor_tensor(out=lsc[:, 2:3], in0=lsc[:, 1:2], in1=lsc[:, 0:1], op=ALU.subtract),
         reads=[lam_b], writes=[lam_b])
    P.op("dve", lambda e: e.tensor_scalar(out=lsc[:, 3:4], in0=lsc[:, 2:3], scalar1=-LAM_INIT0, scalar2=None,
                                          op0=ALU.add), reads=[lam_b], writes=[lam_b])
    cs = sb("cs", [128, NT, 16], F32)
    tri = sb("tri", [128, 128], BF16); nones = sb("nones", [128, 128], BF16)
    mskS = sb("mskS", [128, 4, 512], BF16); mskD = sb("mskD", [128, 4, 512], BF16)
    _st_keep = st
    st = contextlib.ExitStack()
    sb = lambda name, shape, dt: st.enter_context(nc.sbuf_tensor("a_" + name, shape, dt))
    posi = sb("posi", [128, NT], I32); posf = sb("posf", [128, NT], F32)
    ang = sb("ang", [128, NT, 16], F32); invf = sb("invf", [128, NT, 16], F32)
    kf = sb("kf", [128, NT, 16], F32); ki = sb("ki", [128, NT, 16], I32)
    rp_b = P.buf("rope")
    P.dma("sp", lambda e: [e.dma_start(out=posi[:], in_=pos_d.rearrange("(t p) -> p t", p=128),
                                       allow_slow_non_contiguous=True)], stream="pos", writes=[rp_b])
    P.op("dve", lambda e: e.tensor_copy(out=posf[:], in_=posi[:]), reads=[rp_b], writes=[rp_b])
    for i in range(8):
        f = float(np.float32(500000.0) ** np.float32(-(2.0 * i) / 16.0))
        P.op("pool", lambda e, i=i, f=f: e.memset(invf[:, :, i:i + 1], f), writes=[rp_b])
        P.op("pool", lambda e, i=i, f=f: e.memset(invf[:, :, 8 + i:9 + i], f), writes=[rp_b])
    P.op("dve", lambda e: e.tensor_tensor(out=ang[:], in0=invf[:], in1=posf[:].unsqueeze(2).to_broadcast([128, NT, 16]),
                                          op=ALU.mult), reads=[rp_b], writes=[rp_b])
    P.op("dve", lambda e: e.tensor_scalar(out=ang[:, :, 0:8], in0=ang[:, :, 0:8], scalar1=float(np.pi / 2),
                                          scalar2=None, op0=ALU.add), reads=[rp_b], writes=[rp_b])
    P.op("dve", lambda e: e.tensor_scalar(out=kf[:], in0=ang[:], scalar1=float(1.0 / (2 * np.pi)), scalar2=None,
                                          op0=ALU.mult), reads=[rp_b], writes=[rp_b])
    P.op("dve", lambda e: e.tensor_copy(out=ki[:], in_=kf[:]), reads=[rp_b], writes=[rp_b])
    P.op("dve", lambda e: e.tensor_copy(out=kf[:], in_=ki[:]), reads=[rp_b], writes=[rp_b])
    P.op("dve", lambda e: e.scalar_tensor_tensor(out=ang[:], in0=kf[:], scalar=-TWO_PI_HI, in1=ang[:],
                                                 op0=ALU.mult, op1=ALU.add), reads=[rp_b], writes=[rp_b])
    P.op("dve", lambda e: e.scalar_tensor_tensor(out=ang[:], in0=kf[:], scalar=-TWO_PI_LO, in1=ang[:],
                                                 op0=ALU.mult, op1=ALU.add), reads=[rp_b], writes=[rp_b])
    P.op("dve", lambda e: e.tensor_scalar(out=kf[:], in0=ang[:], scalar1=float(np.pi), scalar2=float(-2 * np.pi),
                                          op0=ALU.is_gt, op1=ALU.mult), reads=[rp_b], writes=[rp_b])
    P.op("dve", lambda e: e.tensor_tensor(out=ang[:], in0=ang[:], in1=kf[:], op=ALU.add), reads=[rp_b], writes=[rp_b])
    P.op("dve", lambda e: e.tensor_scalar(out=kf[:], in0=ang[:], scalar1=float(-np.pi), scalar2=float(2 * np.pi),
                                          op0=ALU.is_lt, op1=ALU.mult), reads=[rp_b], writes=[rp_b])
    P.op("dve", lambda e: e.tensor_tensor(out=ang[:], in0=ang[:], in1=kf[:], op=ALU.add), reads=[rp_b], writes=[rp_b])
    P.op("dve", lambda e: e.tensor_scalar(out=ang[:], in0=ang[:], scalar1=float(np.pi), scalar2=float(-np.pi),
                                          op0=ALU.min, op1=ALU.max), reads=[rp_b], writes=[rp_b])
    P.op("act", lambda e: e.activation(out=cs[:], in_=ang[:], func=AF.Sin), reads=[rp_b], writes=[rp_b])

    trif = sb("trif", [128, 512], F32)
    mk_b = P.buf("masks")
    P.op("pool", lambda e: e.memset(trif[:, 0:128], -1.0), writes=[mk_b])
    P.op("pool", lambda e: e.affine_select(out=trif[:, 0:128], in_=trif[:, 0:128], pattern=[[-1, 128]],
                                           compare_op=ALU.is_ge, fill=0.0, base=0, channel_multiplier=1),
         reads=[mk_b], writes=[mk_b])
    P.op("pool", lambda e: e.tensor_copy(out=tri[:], in_=trif[:, 0:128]), reads=[mk_b], writes=[mk_b])
    P.op("pool", lambda e: e.memset(nones[:], -1.0), writes=[mk_b])
    for jd in range(4):
        P.op("pool", lambda e: e.memset(trif[:], 0.0), reads=[mk_b], writes=[mk_b])
        P.op("pool", lambda e, jd=jd: e.affine_select(out=trif[:], in_=trif[:], pattern=[[1, 512]],
                                                      compare_op=ALU.is_gt, fill=NEG, base=-128 * jd,
                                                      channel_multiplier=-1), reads=[mk_b], writes=[mk_b])
        P.op("pool", lambda e, jd=jd: e.tensor_copy(out=mskS[:, jd, :], in_=trif[:]), reads=[mk_b], writes=[mk_b])
        P.op("pool", lambda e: e.memset(trif[:], 0.0), reads=[mk_b], writes=[mk_b])
        for hlf in range(2):
            P.op("pool", lambda e, jd=jd, hlf=hlf: e.affine_select(
                out=trif[hlf * 64:(hlf + 1) * 64, :], in_=trif[hlf * 64:(hlf + 1) * 64, :],
                pattern=[[1, 8], [0, 64]], compare_op=ALU.is_ge, fill=NEG, base=-(2 * jd + hlf),
                channel_multiplier=0), reads=[mk_b], writes=[mk_b])
        P.op("pool", lambda e, jd=jd: e.tensor_copy(out=mskD[:, jd, :], in_=trif[:]), reads=[mk_b], writes=[mk_b])

    P.fence()
    st.close()
    st = _st_keep
    sb = lambda name, shape, dt: st.enter_context(nc.sbuf_tensor("a_" + name, shape, dt))
    xt = [sb("xt%d" % i, [128, D], F32) for i in range(2)]; xt_b = [P.buf("xt%d" % i) for i in range(2)]
    hT = sb("hT0", [128, KC, 512], BF16); hT_b = P.buf("hT0")
    sq = sb("sq0", [128, 256], F32); sq_b = P.buf("sq0")
    ss4 = sb("ss4", [128, 4], F32); ss4_b = P.buf("ss4")
    qk = sb("qk0", [128, 4, 64], F32); qk_b = P.buf("qk0")
    rt = sb("rt0", [128, 4, 4, 8], F32); rt_b = P.buf("rt0")
    qkb = sb("qkb0", [128, 256], BF16); qkb_b = P.buf("qkb0")
    x_v = x_d.rearrange("(t p) d -> p t d", p=128)
    psT = ps[0][:].bitcast(BF16)
    for g in range(NQB):
        for tt in range(4):
            t = g * 4 + tt
            s = t % 2
            P.dma("sp", lambda e, t=t, s=s: [e.dma_start(out=xt[s][:], in_=x_v[:, t, :])], stream=("xt", s),
                  writes=[xt_b[s]])
            emit_norm_tile(P, N, xt[s][:], xt_b[s], ga[:], cst_b, psT, ps_b[0], ident, ident_b,
                           hT[:, :, tt * 128:(tt + 1) * 128], hT_b)
        tok = slice(g * 512, (g + 1) * 512)
        for ci, (dst, dstb, scl) in enumerate(((qTs, qTs_b, 1.0), (kTs, kTs_b, 0.125))):
            pi = 1 + ci
            for kc in range(KC):
                P.op("pe", lambda e, pi=pi, kc=kc, ci=ci: e.matmul(
                    ps[pi][:], lhsT=w6[:, kc, ci * 128:(ci + 1) * 128], rhs=hT[:, kc, :],
                    start=(kc == 0), stop=(kc == KC - 1)), reads=[w6_b, hT_b], writes=[ps_b[pi]])
            P.op("act", lambda e, pi=pi, dst=dst, scl=scl, tok=tok: e.activation(
                out=dst[:, tok], in_=ps[pi][:], func=AF.Copy, scale=scl), reads=[ps_b[pi]], writes=[dstb[g]])
        for tt in range(4):
            t = g * 4 + tt
            pi = 3 + (tt % 2)
            for kc in range(KC):
                P.op("pe", lambda e, pi=pi, kc=kc, tt=tt: e.matmul(
                    ps[pi][:], lhsT=hT[:, kc, tt * 128:(tt + 1) * 128], rhs=w6[:, kc, 256:768],
                    start=(kc == 0), stop=(kc == KC - 1)), reads=[w6_b, hT_b], writes=[ps_b[pi]])
            pp, ppb = ps[pi], ps_b[pi]
            P.op("act", lambda e, pp=pp, t=t: e.activation(out=vs[:, t, :], in_=pp[:, 0:128], func=AF.Copy),
                 reads=[ppb], writes=[vs_b[g]])
            P.op("act", lambda e, pp=pp, t=t: e.activation(out=vd[:, t, 0:128], in_=pp[:, 384:512], func=AF.Copy),
                 reads=[ppb], writes=[vd_b[g]])
            P.op("act", lambda e, pp=pp: e.activation(out=sq[:], in_=pp[:, 128:384], func=AF.Square),
                 reads=[ppb], writes=[sq_b])
            P.op("dve", lambda e: e.tensor_reduce(out=ss4[:], in_=sq[:].rearrange("p (h d) -> p h d", d=64),
                                                  axis=AX.X, op=ALU.add), reads=[sq_b], writes=[ss4_b])
            emit_rstd(P, ss4[:], ss4[:], ss4_b, ss4_b, 1.0 / 64, N.eps[:, 0:1])
            P.op("dve", lambda e, pp=pp: e.tensor_tensor(
                out=qk[:], in0=pp[:, 128:384].rearrange("p (h d) -> p h d", d=64),
                in1=ss4[:].unsqueeze(2).to_broadcast([128, 4, 64]), op=ALU.mult),
                reads=[ppb, ss4_b], writes=[qk_b])
            P.op("dve", lambda e: e.tensor_tensor(out=qk[:], in0=qk[:], in1=gqk[:].rearrange("p (h d) -> p h d", d=64),
                                                  op=ALU.mult), reads=[qk_b, cst_b], writes=[qk_b])
            P.op("dve", lambda e: e.tensor_copy(out=qkb[:], in_=qk[:].rearrange("p h d -> p (h d)")),
                 reads=[qk_b], writes=[qkb_b])
            cosb = cs[:, t, 0:8].unsqueeze(1).to_broadcast([128, 4, 8])
            sinb = cs[:, t, 8:16].unsqueeze(1).to_broadcast([128, 4, 8])
            x1, x2 = qk[:, :, 0:8], qk[:, :, 8:16]
            P.op("dve", lambda e, x1=x1, cosb=cosb: e.tensor_tensor(out=rt[:, 0], in0=x1, in1=cosb, op=ALU.mult),
                 reads=[qk_b, rp_b], writes=[rt_b])
            P.op("dve", lambda e, x2=x2, sinb=sinb: e.tensor_tensor(out=rt[:, 1], in0=x2, in1=sinb, op=ALU.mult),
                 reads=[qk_b, rp_b], writes=[rt_b])
            P.op("dve", lambda e, x2=x2, cosb=cosb: e.tensor_tensor(out=rt[:, 2], in0=x2, in1=cosb, op=ALU.mult),
                 reads=[qk_b, rp_b], writes=[rt_b])
            P.op("dve", lambda e, x1=x1, sinb=sinb: e.tensor_tensor(out=rt[:, 3], in0=x1, in1=sinb, op=ALU.mult),
                 reads=[qk_b, rp_b], writes=[rt_b])
            qv = qkb[:].rearrange("p (h d) -> p h d", d=64)
            P.op("dve", lambda e, qv=qv: e.tensor_tensor(out=qv[:, :, 0:8], in0=rt[:, 0], in1=rt[:, 1],
                                                         op=ALU.subtract), reads=[rt_b], writes=[qkb_b])
            P.op("dve", lambda e, qv=qv: e.tensor_tensor(out=qv[:, :, 8:16], in0=rt[:, 2], in1=rt[:, 3], op=ALU.add),
                 reads=[rt_b], writes=[qkb_b])
            for ci, (dst, dstb) in enumerate(((qTd, qTd_b), (kTd, kTd_b))):
                P.op("pe", lambda e, ci=ci: e.transpose(out=psT[:, ci * 128:(ci + 1) * 128],
                                                        in_=qkb[:, ci * 128:(ci + 1) * 128], identity=ident[:]),
                     reads=[qkb_b, ident_b], writes=[ps_b[0]])
                P.op("act", lambda e, ci=ci, dst=dst, t=t: e.activation(
                    out=dst[:, t * 128:(t + 1) * 128], in_=psT[:, ci * 128:(ci + 1) * 128], func=AF.Copy),
                    reads=[ps_b[0]], writes=[dstb[g]])

    ef = [sb("ef%d" % i, [128, 512], F32) for i in range(2)]; ef_b = [P.buf("ef%d" % i) for i in range(2)]
    spb = [sb("spb%d" % i, [128, 512], BF16) for i in range(2)]; spb_b = [P.buf("spb%d" % i) for i in range(2)]
    wb = [sb("wb%d" % i, [128, 512], BF16) for i in range(2)]; wb_b = [P.buf("wb%d" % i) for i in range(2)]
    slat = sb("slat", [128, 512], BF16); slat_b = P.buf("slat")
    one = sb("one", [128, 1], F32); one_b = P.buf("one")
    P.op("pool", lambda e: e.memset(one[:], 1.0), writes=[one_b])
    it = 0
    for hh in range(2):
        pr = slice(64 * hh, 64 * hh + 64)
        for qb in range(NQB):
            qs = slice(qb * 512, (qb + 1) * 512)
            op_, opb = ps[5], ps_b[5]
            kbs = list(range(4 * qb + 3, -1, -1))
            for n_, kb in enumerate(kbs):
                s = it % 2
                it += 1
                ks = slice(kb * 128, (kb + 1) * 128)
                jd = kb - 4 * qb
                pA, pAb = ps[1 + s], ps_b[1 + s]
                pB, pBb = ps[3 + s], ps_b[3 + s]
                first = n_ == 0

                def scores(dst, dstb, more, pr=pr, ks=ks, qs=qs, jd=jd, kb=kb, qb=qb):
                    P.op("pe", lambda e: e.matmul(dst[:], lhsT=kTs[pr, ks], rhs=qTs[pr, qs], start=True,
                                                  stop=(jd < 0 and not more)),
                         reads=[kTs_b[kb // 4], qTs_b[qb]], writes=[dstb])
                    if jd >= 0:
                        P.op("pe", lambda e: e.matmul(dst[:], lhsT=ident[:], rhs=mskS[:, jd, :], start=False,
                                                      stop=not more), reads=[ident_b, mk_b], writes=[dstb])
                scores(pA, pAb, False)
                P.op("act", lambda e, s=s, pA=pA: e.activation(out=ef[s][:], in_=pA[:], func=AF.Exp),
                     reads=[pAb], writes=[ef_b[s]])
                P.op("act", lambda e, s=s: e.activation(out=spb[s][:], in_=ef[s][:], func=AF.Ln, bias=one[:, 0:1]),
                     reads=[ef_b[s], one_b], writes=[spb_b[s]])
                scores(pB, pBb, True)
                P.op("pe", lambda e, s=s, pB=pB, first=first: e.matmul(pB[:], lhsT=tri[:], rhs=spb[s][:], start=False,
                                                                       stop=first),
                     reads=[mk_b, spb_b[s]], writes=[pBb])
                if not first:
                    P.op("pe", lambda e, pB=pB: e.matmul(pB[:], lhsT=nones[:], rhs=slat[:], start=False, stop=True),
                         reads=[mk_b, slat_b], writes=[pBb])
                P.op("act", lambda e, s=s, pB=pB: e.activation(out=wb[s][:], in_=pB[:], func=AF.Exp),
                     reads=[pBb], writes=[wb_b[s]])
                if first:
                    P.op("dve", lambda e, s=s: e.tensor_copy(out=slat[:], in_=spb[s][:]), reads=[spb_b[s]],
                         writes=[slat_b])
                elif n_ < len(kbs) - 1:
                    P.op("dve", lambda e, s=s: e.tensor_tensor(out=slat[:], in0=slat[:], in1=spb[s][:], op=ALU.add),
                         reads=[spb_b[s], slat_b], writes=[slat_b])
                for i in range(4):
                    last = n_ == len(kbs) - 1
                    P.op("pe", lambda e, s=s, i=i, kb=kb, hh=hh, first=first, last=last: e.matmul(
                        op_[:, i * 64:(i + 1) * 64], lhsT=wb[s][:, i * 128:(i + 1) * 128],
                        rhs=vs[:, kb, hh * 64:(hh + 1) * 64], start=(first and i == 0),
                        stop=(last and i == 3), skip_group_check=True),
                        reads=[wb_b[s], vs_b[kb // 4]], writes=[opb])
            P.op("dve", lambda e, qb=qb, hh=hh: e.tensor_copy(
                out=osb[:, qb * 4:(qb + 1) * 4, hh * 64:(hh + 1) * 64],
                in_=op_[:, 0:256].rearrange("p (i d) -> p i d", d=64)), reads=[opb], writes=[osb_b[qb]])

    t1 = sb("t1", [128, 128], F32); t1_b = P.buf("t1")
    rd = sb("rd", [128, 4], F32); rd_b = P.buf("rd")
    for qb in range(NQB):
        qs = slice(qb * 512, (qb + 1) * 512)
        nkb = 4 * qb + 4
        obank = {(0, 0): 4, (0, 1): 5, (1, 0): 6, (1, 1): 7}
        for kb in range(nkb):
            ks = slice(kb * 128, (kb + 1) * 128)
            jd = kb - 4 * qb
            for mm in range(2):
                s = it % 2
                it += 1
                pr = slice(64 * mm, 64 * mm + 64)
                pA, pAb = ps[1 + s], ps_b[1 + s]
                P.op("pe", lambda e, pA=pA, pr=pr, ks=ks, qs=qs, jd=jd: e.matmul(
                    pA[:], lhsT=kTd[pr, ks], rhs=qTd[pr, qs], start=True, stop=(jd < 0)),
                     reads=[kTd_b[kb // 4], qTd_b[qb]], writes=[pAb])
                if jd >= 0:
                    P.op("pe", lambda e, pA=pA, jd=jd: e.matmul(pA[:], lhsT=ident[:], rhs=mskD[:, jd, :], start=False,
                                                                stop=True), reads=[ident_b, mk_b], writes=[pAb])
                P.op("act", lambda e, s=s, pA=pA: e.activation(out=wb[s][:], in_=pA[:], func=AF.Exp),
                     reads=[pAb], writes=[wb_b[s]])
                for i in range(4):
                    ob = obank[(mm, i // 2)]
                    c0 = (i % 2) * 129
                    P.op("pe", lambda e, s=s, i=i, kb=kb, ob=ob, c0=c0, nkb=nkb: e.matmul(
                        ps[ob][:, c0:c0 + 129], lhsT=wb[s][:, i * 128:(i + 1) * 128], rhs=vd[:, kb, 0:129],
                        start=(kb == 0 and i % 2 == 0), stop=(kb == nkb - 1 and i % 2 == 1), skip_group_check=True),
                        reads=[wb_b[s], vd_b[kb // 4]], writes=[ps_b[ob]])
        for i in range(4):
            t = qb * 4 + i
            c0 = (i % 2) * 129
            o1, o1b = ps[obank[(0, i // 2)]], ps_b[obank[(0, i // 2)]]
            o2, o2b = ps[obank[(1, i // 2)]], ps_b[obank[(1, i // 2)]]
            P.op("dve", lambda e, o1=o1, c0=c0: e.reciprocal(out=rd[:, 0:1], in_=o1[:, c0 + 128:c0 + 129]),
                 reads=[o1b], writes=[rd_b])
            P.op("dve", lambda e, o2=o2, c0=c0: e.reciprocal(out=rd[:, 1:2], in_=o2[:, c0 + 128:c0 + 129]),
                 reads=[o2b], writes=[rd_b])
            P.op("dve", lambda e: e.tensor_tensor(out=rd[:, 1:2], in0=rd[:, 1:2], in1=lsc[:, 3:4], op=ALU.mult),
                 reads=[rd_b, lam_b], writes=[rd_b])
            P.op("dve", lambda e, o1=o1, c0=c0: e.tensor_scalar(out=t1[:], in0=o1[:, c0:c0 + 128], scalar1=rd[:, 0:1],
                                                                scalar2=None, op0=ALU.mult),
                 reads=[o1b, rd_b], writes=[t1_b])
            P.op("dve", lambda e, o2=o2, c0=c0: e.scalar_tensor_tensor(
                out=t1[:], in0=o2[:, c0:c0 + 128], scalar=rd[:, 1:2], in1=t1[:], op0=ALU.mult, op1=ALU.add),
                reads=[o2b, rd_b, t1_b], writes=[t1_b])
            P.op("act", lambda e: e.activation(out=N.junk[:, 0:128], in_=t1[:], func=AF.Square, accum_out=rd[:, 2:3]),
                 reads=[t1_b], writes=[N.junk_b, rd_b])
            emit_rstd(P, rd[:, 2:3], rd[:, 3:4], rd_b, rd_b, 1.0 / 128, N.eps[:, 0:1])
            P.op("dve", lambda e, t=t: e.scalar_tensor_tensor(
                out=osb[:, t, 128:256], in0=t1[:], scalar=rd[:, 3:4], in1=subg[:], op0=ALU.mult, op1=ALU.mult),
                reads=[t1_b, rd_b, cst_b], writes=[osb_b[qb]])

    oTs = [sb("oTs%d" % i, [128, 2, 512], BF16) for i in range(2)]; oTs_b = [P.buf("oTs%d" % i) for i in range(2)]
    oT_v = oT_d.rearrange("(c p) s -> p c s", p=128)
    for qb in range(NQB):
        s = qb % 2
        for i in range(4):
            t = qb * 4 + i
            for c in range(2):
                P.op("pe", lambda e, t=t, c=c, i=i: e.transpose(
                    out=psT[:, (i % 2) * 256 + c * 128:(i % 2) * 256 + (c + 1) * 128],
                    in_=osb[:, t, c * 128:(c + 1) * 128], identity=ident[:]),
                    reads=[osb_b[qb], ident_b], writes=[ps_b[0]])
            if i % 2 == 1:
                P.op("act", lambda e, s=s, i=i: e.activation(
                    out=oTs[s][:, :, (i - 1) * 128:(i + 1) * 128].rearrange("p c (i t) -> p i c t", i=2),
                    in_=psT[:, 0:512].rearrange("p (i c t) -> p i c t", i=2, c=2), func=AF.Copy),
                    reads=[ps_b[0]], writes=[oTs_b[s]])
        P.dma("sp", lambda e, s=s, qb=qb: [e.dma_start(out=oT_v[:, :, qb * 512:(qb + 1) * 512], in_=oTs[s][:])],
              stream=("oT", s), reads=[oTs_b[s]])
    P.wait_all("sp", oTs_b)


def build_a(NT=64):
    nc = bass.Bass("TRN2", target_bir_lowering=False)
    S = NT * 128
    x_d = nc.dram_tensor("x", [S, D], F32, kind="ExternalInput").ap()
    pos_d = nc.dram_tensor("pos", [S], I32, kind="ExternalInput").ap()
    ga_d = nc.dram_tensor("ga", [D], F32, kind="ExternalInput").ap()
    w6_d = nc.dram_tensor("w6", [D, 768], F32, kind="ExternalInput").ap()
    gqk_d = nc.dram_tensor("gqk", [256], F32, kind="ExternalInput").ap()
    lam_d = nc.dram_tensor("lam", [256], F32, kind="ExternalInput").ap()
    sub_d = nc.dram_tensor("sub", [128], F32, kind="ExternalInput").ap()
    oT_d = nc.dram_tensor("oT", [256, S], BF16, kind="ExternalOutput").ap()
    P = Prog(nc)
    with contextlib.ExitStack() as st:
        ps, ps_b = alloc_psum(P, nc, st)
        ident, ident_b, identf = make_ident(P, nc, st)
        emit_l0_attn(P, nc, st, ps, ps_b, ident, ident_b, x_d, pos_d, ga_d, w6_d, gqk_d, lam_d, sub_d, oT_d, NT)
        P.emit(st)
    return nc


def l0_core_inputs(x, positions, ga, w_in, qn, kn, l4, subln, b, j, S):
    cols = np.concatenate([np.arange(128 * j, 128 * j + 128) + off for off in (0, 512, 1024, 1536, 2048, 2560)])
    return {"x": np.ascontiguousarray(x[b, :S]), "pos": np.ascontiguousarray(positions[b, :S]).astype(np.int32),
            "ga": ga, "w6": np.ascontiguousarray(w_in[:, cols]),
            "gqk": np.concatenate([qn, qn, kn, kn]).astype(np.float32), "lam": np.concatenate(l4).astype(np.float32),
            "sub": subln}


_NC_CACHE = {}


def _get(name, fn):
    if name not in _NC_CACHE:
        _NC_CACHE[name] = fn()
    return _NC_CACHE[name]


def kernel(x, positions, ev_attn_norm, ev_w_in, ev_q_norm, ev_k_norm, ev_lambda_q1, ev_lambda_k1,
           ev_lambda_q2, ev_lambda_k2, ev_subln, ev_w_out, ev_ffn_norm, ev_w_gate, ev_w_up, ev_w_down,
           od_attn_norm, od_w_qkv, od_q_norm, od_k_norm, od_rel_bias, od_w_out, od_ffn_norm, od_router,
           od_we_gate, od_we_up, od_we_down):
    f32 = lambda a: np.ascontiguousarray(np.asarray(a), dtype=np.float32)
    x = f32(x)
    positions = np.asarray(positions)
    B, S, _ = x.shape
    cores = list(range(NCORES))
    nca = _get("a", lambda: build_a(S // 128))
    l4 = [f32(ev_lambda_q1)[0], f32(ev_lambda_k1)[0], f32(ev_lambda_q2)[0], f32(ev_lambda_k2)[0]]
    in_a = [l0_core_inputs(x, positions, f32(ev_attn_norm)[0], f32(ev_w_in)[0], f32(ev_q_norm)[0], f32(ev_k_norm)[0],
                           l4, f32(ev_subln)[0], c // 4, c % 4, S) for c in cores]
    ra = run_bass_kernel_spmd(nca, in_a, core_ids=cores).results
    mT = np.zeros((B, D, S), dtype=ml_dtypes.bfloat16)
    for c in cores:
        b, j = c // 4, c % 4
        o = np.asarray(ra[c]["oT"])
        mT[b, 128 * j:128 * j + 128] = o[0:128]
        mT[b, 512 + 128 * j:512 + 128 * j + 128] = o[128:256]
    ncb1 = _get("b1", lambda: build_b1(16))
    in_b1 = []
    for c in cores:
        b, j = c // 4, c % 4
        tok = slice(j * 2048, (j + 1) * 2048)
        in_b1.append({"x": np.ascontiguousarray(x[b, tok]), "mT": np.ascontiguousarray(mT[b][:, tok]),
                      "w_out": f32(ev_w_out)[0], "g": f32(ev_ffn_norm)[0], "w_gate": f32(ev_w_gate)[0],
                      "w_up": f32(ev_w_up)[0], "w_down": f32(ev_w_down)[0]})
    rb1 = run_bass_kernel_spmd(ncb1, in_b1, core_ids=cores).results
    x2 = np.stack([np.concatenate([np.asarray(rb1[b * 4 + j]["y"]) for j in range(4)], axis=0) for b in range(B)])
    ncb2 = _get("b2", lambda: build_b2(16))
    idx = band_bias_index()
    rel = f32(od_rel_bias)[0]
    bm = np.ascontiguousarray(rel[:, idx[3:5]].transpose(2, 0, 1, 3))
    cb = np.ascontiguousarray(rel[:, 256])
    in_b2 = []
    for c in cores:
        b, j = c // 4, c % 4
        tok = slice(j * 2048, (j + 1) * 2048)
        xh = x2[b, j * 2048 - 512:j * 2048] if j > 0 else np.zeros((512, D), np.float32)
        in_b2.append({"x": np.ascontiguousarray(x2[b, tok]), "xh": np.ascontiguousarray(xh),
                      "halo": np.full((128, 1), 0.0 if j > 0 else NEG, np.float32),
                      "ga": f32(od_attn_norm)[0], "wqkv": f32(od_w_qkv)[0],
                      "gq": np.tile(f32(od_q_norm)[0], 8), "gk": np.tile(f32(od_k_norm)[0], 8),
                      "bm": bm, "cb": cb, "wo": f32(od_w_out)[0], "gf": f32(od_ffn_norm)[0],
                      "wr": f32(od_router)[0], "weg": f32(od_we_gate)[0], "weu": f32(od_we_up)[0],
                      "wed": f32(od_we_down)[0]})
    rb2 = run_bass_kernel_spmd(ncb2, in_b2, core_ids=cores).results
    out = np.stack([np.concatenate([np.asarray(rb2[b * 4 + j]["y"]) for j in range(4)], axis=0) for b in range(B)])
    return out.astype(np.float32)
```
